# Optimizing a Trainium2 kernel written in Bass

```python
import jax
import jax.numpy as jnp
from jax import lax
import numpy as np

D_MODEL = 1024
BATCH = 8
SEQ = 4096
DEPTH = 1

HEAD_DIM = 64
MIX_WIDTH = D_MODEL
N_FOX_HEADS = MIX_WIDTH // (2 * HEAD_DIM)
N_SWA_HEADS = MIX_WIDTH // (2 * HEAD_DIM)
N_SWA_KV_HEADS = 2
SWA_GROUP = N_SWA_HEADS // N_SWA_KV_HEADS
WINDOW = 128
Q_BLOCK = 128
ROPE_THETA = 10000.0
N_MEM = 256
N_XATTN_HEADS = 4
XATTN_HEAD_DIM = D_MODEL // N_XATTN_HEADS
N_GROUPS = 4
EXPERTS_PER_GROUP = 8
N_EXPERTS = N_GROUPS * EXPERTS_PER_GROUP
TOP_K_IN_GROUP = 2
D_EXPERT = 512
ROW_BLOCK = 128
LN_EPS = 1e-5
FOX_W = N_FOX_HEADS * HEAD_DIM
SWA_Q_W = N_SWA_HEADS * HEAD_DIM
SWA_KV_W = N_SWA_KV_HEADS * HEAD_DIM
IN_SIZES = (FOX_W, FOX_W, FOX_W, N_FOX_HEADS, SWA_Q_W, SWA_KV_W, SWA_KV_W)
IN_PROJ_W = sum(IN_SIZES)

kernel_name = 'hybrid_fox_swa_sink_hmoe_deepnorm'


def layer_norm(x, g, b):
    xf = x.astype(jnp.float32)
    mu = jnp.mean(xf, axis=-1, keepdims=True)
    var = jnp.mean(jnp.square(xf - mu), axis=-1, keepdims=True)
    y = (xf - mu) * lax.rsqrt(var + LN_EPS) * g.astype(jnp.float32) + b.astype(jnp.float32)
    return y.astype(x.dtype)


def rope(x, positions):
    half = x.shape[-1] // 2
    inv_freq = ROPE_THETA ** (-jnp.arange(half, dtype=jnp.float32) / half)
    ang = positions.astype(jnp.float32)[..., None] * inv_freq
    cos = jnp.cos(ang)[:, :, None, :]
    sin = jnp.sin(ang)[:, :, None, :]
    xf = x.astype(jnp.float32)
    x1, x2 = xf[..., :half], xf[..., half:]
    return jnp.concatenate([x1 * cos - x2 * sin, x2 * cos + x1 * sin], axis=-1).astype(x.dtype)


def forgetting_attention(q, k, v, log_f):
    B, S, H, dh = q.shape
    nb = S // Q_BLOCK
    scale = dh ** -0.5
    c = jnp.cumsum(log_f, axis=1).transpose(0, 2, 1)
    kh = k.transpose(0, 2, 1, 3)
    vh = v.transpose(0, 2, 1, 3)
    qb = q.reshape(B, nb, Q_BLOCK, H, dh).transpose(1, 0, 3, 2, 4)
    cb = c.reshape(B, H, nb, Q_BLOCK).transpose(2, 0, 1, 3)
    key_pos = jnp.arange(S)

    def block(args):
        q_blk, c_blk, i = args
        s = jnp.einsum('bhqd,bhsd->bhqs', q_blk, kh).astype(jnp.float32) * scale
        s = s + c_blk[..., None] - c[:, :, None, :]
        qpos = i * Q_BLOCK + jnp.arange(Q_BLOCK)
        causal = key_pos[None, :] <= qpos[:, None]
        s = jnp.where(causal, s, -jnp.inf)
        p = jax.nn.softmax(s, axis=-1)
        return jnp.einsum('bhqs,bhsd->bhqd', p.astype(vh.dtype), vh)

    o = lax.map(block, (qb, cb, jnp.arange(nb)))
    return o.transpose(1, 0, 3, 2, 4).reshape(B, S, H * dh)


def sliding_window_sink_attention(q, k, v, sinks):
    B, S, _, dh = q.shape
    nb = S // WINDOW
    scale = dh ** -0.5
    qb = q.reshape(B, nb, WINDOW, N_SWA_KV_HEADS, SWA_GROUP, dh)

    def band(t):
        t = t.reshape(B, nb, WINDOW, N_SWA_KV_HEADS, dh)
        prev = jnp.concatenate([jnp.zeros_like(t[:, :1]), t[:, :-1]], axis=1)
        return jnp.concatenate([prev, t], axis=2)

    kb, vb = band(k), band(v)
    s = jnp.einsum('bnqkgd,bnskd->bnkgqs', qb, kb).astype(jnp.float32) * scale
    blk = jnp.arange(nb)[:, None, None] * WINDOW
    qpos = blk + jnp.arange(WINDOW)[None, :, None]
    kpos = blk + jnp.arange(2 * WINDOW)[None, None, :] - WINDOW
    valid = (kpos <= qpos) & (qpos - kpos < WINDOW) & (kpos >= 0)
    s = jnp.where(valid[None, :, None, None], s, -jnp.inf)
    sink = jnp.broadcast_to(
        sinks.astype(jnp.float32).reshape(1, 1, N_SWA_KV_HEADS, SWA_GROUP, 1, 1),
        s.shape[:-1] + (1,))
    p = jax.nn.softmax(jnp.concatenate([s, sink], axis=-1), axis=-1)[..., :-1]
    o = jnp.einsum('bnkgqs,bnskd->bnqkgd', p.astype(vb.dtype), vb)
    return o.reshape(B, S, N_SWA_HEADS * dh)


def hybrid_mixer(h, positions, w_in, b_forget, sinks, w_out):
    B, S, _ = h.shape
    z = h @ w_in
    splits = [int(v) for v in np.cumsum(IN_SIZES)[:-1]]
    q_f, k_f, v_f, f_logit, q_s, k_s, v_s = jnp.split(z, splits, axis=-1)
    log_f = jax.nn.log_sigmoid((f_logit + b_forget).astype(jnp.float32))
    o_fox = forgetting_attention(
        q_f.reshape(B, S, N_FOX_HEADS, HEAD_DIM),
        k_f.reshape(B, S, N_FOX_HEADS, HEAD_DIM),
        v_f.reshape(B, S, N_FOX_HEADS, HEAD_DIM), log_f)
    q_s = rope(q_s.reshape(B, S, N_SWA_HEADS, HEAD_DIM), positions)
    k_s = rope(k_s.reshape(B, S, N_SWA_KV_HEADS, HEAD_DIM), positions)
    v_s = v_s.reshape(B, S, N_SWA_KV_HEADS, HEAD_DIM)
    o_swa = sliding_window_sink_attention(q_s, k_s, v_s, sinks)
    return jnp.concatenate([o_fox, o_swa], axis=-1) @ w_out


def memory_cross_attention(h, mem, w_q, w_kv, w_out):
    B, S, D = h.shape
    M = mem.shape[1]
    q = (h @ w_q).reshape(B, S, N_XATTN_HEADS, XATTN_HEAD_DIM)
    k, v = jnp.split(mem @ w_kv, 2, axis=-1)
    k = k.reshape(B, M, N_XATTN_HEADS, XATTN_HEAD_DIM)
    v = v.reshape(B, M, N_XATTN_HEADS, XATTN_HEAD_DIM)
    s = jnp.einsum('bqhd,bmhd->bhqm', q, k).astype(jnp.float32) * (XATTN_HEAD_DIM ** -0.5)
    p = jax.nn.softmax(s, axis=-1)
    o = jnp.einsum('bhqm,bmhd->bqhd', p.astype(v.dtype), v).reshape(B, S, D)
    return o @ w_out


def hierarchical_moe(h, w_rg, b_rg, w_re, b_re, w_gate, w_up, w_down):
    B, S, D = h.shape
    T = B * S
    xt = h.reshape(T, D)
    p_g = jax.nn.softmax((xt @ w_rg + b_rg).astype(jnp.float32), axis=-1)
    g_val, g_idx = lax.top_k(p_g, 1)
    g_val, g_idx = g_val[:, 0], g_idx[:, 0]
    el = jnp.einsum('td,gde->tge', xt, w_re) + b_re
    sel = jnp.take_along_axis(el, g_idx[:, None, None], axis=1)[:, 0].astype(jnp.float32)
    e_val, e_idx = lax.top_k(sel, TOP_K_IN_GROUP)
    gate = (g_val[:, None] * jax.nn.softmax(e_val, axis=-1)).astype(h.dtype)
    expert_id = g_idx[:, None] * EXPERTS_PER_GROUP + e_idx

    A = T * TOP_K_IN_GROUP
    flat_e = expert_id.reshape(-1).astype(jnp.int32)
    flat_tok = jnp.repeat(jnp.arange(T, dtype=jnp.int32), TOP_K_IN_GROUP)
    flat_w = gate.reshape(-1)
    order = jnp.argsort(flat_e, stable=True)
    se, st, sw = flat_e[order], flat_tok[order], flat_w[order]
    counts = jax.ops.segment_sum(jnp.ones_like(flat_e), flat_e, num_segments=N_EXPERTS)
    start = jnp.cumsum(counts) - counts
    padded = ((counts + ROW_BLOCK - 1) // ROW_BLOCK) * ROW_BLOCK
    pad_end = jnp.cumsum(padded)
    pad_start = pad_end - padded
    dest = pad_start[se] + (jnp.arange(A, dtype=jnp.int32) - start[se])
    P = A + N_EXPERTS * ROW_BLOCK
    NB = P // ROW_BLOCK
    row_tok = jnp.full((P,), T, dtype=jnp.int32).at[dest].set(st)
    row_w = jnp.zeros((P,), dtype=h.dtype).at[dest].set(sw)
    block_e = jnp.clip(jnp.searchsorted(pad_end, jnp.arange(NB, dtype=jnp.int32) * ROW_BLOCK,
                                        side='right'), 0, N_EXPERTS - 1)
    xt_pad = jnp.concatenate([xt, jnp.zeros((1, D), dtype=xt.dtype)], axis=0)
    xr = xt_pad[row_tok].reshape(NB, ROW_BLOCK, D)

    def expert_block(args):
        xb, e = args
        a = jax.nn.silu(xb @ w_gate[e]) * (xb @ w_up[e])
        return a @ w_down[e]

    yr = lax.map(expert_block, (xr, block_e)).reshape(P, D)
    y = jax.ops.segment_sum(yr * row_w[:, None], row_tok, num_segments=T + 1)[:T]
    return y.reshape(B, S, D)


def setup_inputs(seed: int = 0) -> dict:
    key = jax.random.key(seed)
    ks = jax.random.split(key, 24)
    f32 = jnp.float32

    def nrm(k, shape, s):
        return jax.random.normal(k, shape, f32) * s

    beta = (8.0 * DEPTH) ** -0.25
    d_in = D_MODEL ** -0.5
    L = DEPTH
    x = jax.random.normal(ks[0], (BATCH, SEQ, D_MODEL), f32)
    mem = jax.random.normal(ks[1], (BATCH, N_MEM, D_MODEL), f32)
    start = jax.random.randint(ks[2], (BATCH, 1), 0, 1024, dtype=jnp.int32)
    positions = start + jnp.arange(SEQ, dtype=jnp.int32)[None, :]
    w_in = nrm(ks[3], (L, D_MODEL, IN_PROJ_W), d_in)
    b_forget = jnp.linspace(1.0, 6.0, N_FOX_HEADS, dtype=f32)[None, :] + nrm(ks[4], (L, N_FOX_HEADS), 0.01)
    sinks = nrm(ks[5], (L, N_SWA_HEADS), 0.5)
    w_mix_out = nrm(ks[6], (L, MIX_WIDTH, D_MODEL), MIX_WIDTH ** -0.5 * beta)
    ln_mix_g = 1.0 + nrm(ks[7], (L, D_MODEL), 0.02)
    ln_mix_b = nrm(ks[8], (L, D_MODEL), 0.02)
    w_xq = nrm(ks[9], (L, D_MODEL, D_MODEL), d_in)
    w_xkv = nrm(ks[10], (L, D_MODEL, 2 * D_MODEL), d_in)
    w_xout = nrm(ks[11], (L, D_MODEL, D_MODEL), d_in * beta)
    ln_x_g = 1.0 + nrm(ks[12], (L, D_MODEL), 0.02)
    ln_x_b = nrm(ks[13], (L, D_MODEL), 0.02)
    w_route_group = nrm(ks[14], (L, D_MODEL, N_GROUPS), d_in)
    b_route_group = nrm(ks[15], (L, N_GROUPS), 0.01)
    w_route_expert = nrm(ks[16], (L, N_GROUPS, D_MODEL, EXPERTS_PER_GROUP), d_in)
    b_route_expert = nrm(ks[17], (L, N_GROUPS, EXPERTS_PER_GROUP), 0.01)
    w_exp_gate = nrm(ks[18], (L, N_EXPERTS, D_MODEL, D_EXPERT), d_in)
    w_exp_up = nrm(ks[19], (L, N_EXPERTS, D_MODEL, D_EXPERT), d_in)
    w_exp_down = nrm(ks[20], (L, N_EXPERTS, D_EXPERT, D_MODEL), D_EXPERT ** -0.5 * beta)
    ln_ffn_g = 1.0 + nrm(ks[21], (L, D_MODEL), 0.02)
    ln_ffn_b = nrm(ks[22], (L, D_MODEL), 0.02)
    return {'x': x, 'mem': mem, 'positions': positions, 'w_in': w_in, 'b_forget': b_forget,
            'sinks': sinks, 'w_mix_out': w_mix_out, 'ln_mix_g': ln_mix_g, 'ln_mix_b': ln_mix_b,
            'w_xq': w_xq, 'w_xkv': w_xkv, 'w_xout': w_xout, 'ln_x_g': ln_x_g, 'ln_x_b': ln_x_b,
            'w_route_group': w_route_group, 'b_route_group': b_route_group,
            'w_route_expert': w_route_expert, 'b_route_expert': b_route_expert,
            'w_exp_gate': w_exp_gate, 'w_exp_up': w_exp_up, 'w_exp_down': w_exp_down,
            'ln_ffn_g': ln_ffn_g, 'ln_ffn_b': ln_ffn_b}


def reference(x, mem, positions, w_in, b_forget, sinks, w_mix_out, ln_mix_g, ln_mix_b,
              w_xq, w_xkv, w_xout, ln_x_g, ln_x_b, w_route_group, b_route_group,
              w_route_expert, b_route_expert, w_exp_gate, w_exp_up, w_exp_down,
              ln_ffn_g, ln_ffn_b):
    alpha = (2.0 * DEPTH) ** 0.25
    h = x
    for l in range(DEPTH):
        h = layer_norm(alpha * h + hybrid_mixer(h, positions, w_in[l], b_forget[l], sinks[l], w_mix_out[l]),
                       ln_mix_g[l], ln_mix_b[l])
        h = layer_norm(alpha * h + memory_cross_attention(h, mem, w_xq[l], w_xkv[l], w_xout[l]),
                       ln_x_g[l], ln_x_b[l])
        h = layer_norm(alpha * h + hierarchical_moe(h, w_route_group[l], b_route_group[l],
                                                    w_route_expert[l], b_route_expert[l],
                                                    w_exp_gate[l], w_exp_up[l], w_exp_down[l]),
                       ln_ffn_g[l], ln_ffn_b[l])
    return h
```

```python
import contextlib
import numpy as np
import concourse.bass as bass
import concourse.mybir as mybir
from concourse.bass_utils import run_bass_kernel_spmd

F32 = mybir.dt.float32
BF16 = mybir.dt.bfloat16
I32 = mybir.dt.int32
AF = mybir.ActivationFunctionType
ALU = mybir.AluOpType
AX = mybir.AxisListType

ENGS = ("pe", "act", "dve", "pool", "sp")
S_TOK = 4096
D = 1024
NT = 32
CAP = 512
NE = 32
import os
STOP = os.environ.get('KSTOP', '')
ALPHA = float(2.0 ** 0.25)
EPS = 1e-5
TWO_PI = 6.283185307179586


class Buf:
    __slots__ = ("name", "last_w", "reads")

    def __init__(self, name=""):
        self.name = name
        self.last_w = None
        self.reads = {}


class Sched:
    def __init__(self, nc, stack, n_dma_sems=8):
        self.nc = nc
        self.stack = stack
        self.streams = {e: [] for e in ENGS}
        self.sems = {}
        self.eng_sem = {}
        self.eng_cnt = {}
        self.waited = {e: {} for e in ENGS}
        self.n_dma_sems = n_dma_sems
        self.dma_sems = {}
        self.dma_vals = {}
        self.dma_rr = {}
        self.phase_id = 0
        self.sem_phase = {}
        self.n_ops = 0
        self._new_phase_sems()

    def _mk_sem(self, key):
        h = self.stack.enter_context(self.nc.semaphore(key))
        self.sems[key] = h
        self.sem_phase[key] = self.phase_id
        return key

    def _new_phase_sems(self):
        p = self.phase_id
        for e in ENGS:
            self.eng_sem[e] = self._mk_sem(f"s{p}_{e}")
            self.eng_cnt[e] = 0
        for e in ("sp", "pool", "act"):
            self.dma_sems[e] = [self._mk_sem(f"d{p}_{e}{i}") for i in range(self.n_dma_sems)]
            self.dma_vals[e] = [0] * self.n_dma_sems
            self.dma_rr[e] = 0

    def _wait(self, e, tok):
        if tok is None:
            return
        key, val = tok
        if self.waited[e].get(key, 0) >= val:
            return
        self.waited[e][key] = val
        self.streams[e].append(("wait", key, val))

    def _deps(self, e, reads, writes, extra):
        deps = []
        for b in reads:
            if b.last_w is not None:
                deps.append(b.last_w)
        for b in writes:
            if b.last_w is not None:
                deps.append(b.last_w)
            for k, v in b.reads.items():
                deps.append((k, v))
        for t in extra:
            if t is not None:
                deps.append(t)
        own = self.eng_sem[e]
        for t in deps:
            if e == "pe" and t[0] == own:
                continue
            self._wait(e, t)

    def _commit(self, tok, reads, writes):
        k, v = tok
        for b in reads:
            if b.reads.get(k, 0) < v:
                b.reads[k] = v
        for b in writes:
            b.last_w = tok
            b.reads = {}

    def op(self, e, fn, reads=(), writes=(), deps=()):
        self._deps(e, reads, writes, deps)
        self.eng_cnt[e] += 1
        tok = (self.eng_sem[e], self.eng_cnt[e])
        self.streams[e].append(("op", fn, tok[0], 1))
        self._commit(tok, reads, writes)
        self.n_ops += 1
        return tok

    def dma(self, e, fn, reads=(), writes=(), deps=()):
        self._deps(e, reads, writes, deps)
        i = self.dma_rr[e]
        self.dma_rr[e] = (i + 1) % self.n_dma_sems
        key = self.dma_sems[e][i]
        if self.dma_vals[e][i] > 0:
            self._wait(e, (key, self.dma_vals[e][i]))
        self.dma_vals[e][i] += 16
        tok = (key, self.dma_vals[e][i])
        self.streams[e].append(("op", fn, key, 16))
        self._commit(tok, reads, writes)
        self.n_ops += 1
        return tok

    def flush(self):
        nc = self.nc
        finals = []
        for e in ENGS:
            if self.eng_cnt[e] > 0:
                finals.append((self.eng_sem[e], self.eng_cnt[e]))
        for e in ("sp", "pool", "act"):
            for i in range(self.n_dma_sems):
                if self.dma_vals[e][i] > 0:
                    finals.append((self.dma_sems[e][i], self.dma_vals[e][i]))
        for e in ENGS:
            for t in finals:
                self._wait(e, t)
        streams = self.streams
        sems = self.sems

        def replay(eng, lst):
            for it in lst:
                if it[0] == "wait":
                    eng.wait_ge(sems[it[1]], it[2])
                else:
                    ins = it[1](eng)
                    ins.then_inc(sems[it[2]], it[3])

        with nc.Block() as block:
            @block.tensor
            def _(eng):
                replay(eng, streams["pe"])

            @block.scalar
            def _(eng):
                replay(eng, streams["act"])

            @block.vector
            def _(eng):
                replay(eng, streams["dve"])

            @block.gpsimd
            def _(eng):
                replay(eng, streams["pool"])

            @block.sync
            def _(eng):
                replay(eng, streams["sp"])

        self.streams = {e: [] for e in ENGS}


class Rot:
    def __init__(self, items):
        self.items = items
        self.i = 0

    def next(self):
        it = self.items[self.i]
        self.i = (self.i + 1) % len(self.items)
        return it


def build_program(n_phases=99, debug=False):
    nc = bass.Bass("TRN2", target_bir_lowering=False)

    declared = []

    def din(name, shape, dt=F32, ph=0):
        if n_phases < ph:
            return None
        declared.append(name)
        return nc.dram_tensor(name, list(shape), dt, kind="ExternalInput").ap()

    def dscr(name, shape, dt):
        return nc.dram_tensor(name, list(shape), dt, kind="Internal").ap()

    xT = din("xT", [D, S_TOK], ph=1)
    x_tok = din("x_tok", [S_TOK, D], ph=3)
    memT = din("memT", [D, 256], ph=3)
    pos = din("pos", [1, S_TOK], I32)
    w_fm = din("w_fm", [D, 2304], ph=1)
    w_tm = din("w_tm", [D, 648], ph=1)
    b_forget = din("b_forget", [1, 8], ph=1)
    sinks = din("sinks", [1, 8])
    w_mix_out = din("w_mix_out", [D, D], ph=3)
    w_xq = din("w_xq", [D, D], ph=3)
    w_xkv = din("w_xkv", [D, 2 * D], ph=3)
    w_xout = din("w_xout", [D, D], ph=3)
    lnp = din("lnp", [6, D], ph=3)
    w_r = din("w_r", [D, 36], ph=3)
    b_r = din("b_r", [1, 36], ph=3)
    w_gate = din("w_gate", [NE, D, 512], ph=4)
    w_up = din("w_up", [NE, D, 512], ph=4)
    w_down = din("w_down", [NE, 512, D], ph=4)
    cinv = din("cinv", [128, 1])
    csgn = din("csgn", [128, 1])
    out = nc.dram_tensor("out", [S_TOK, D], F32, kind="ExternalOutput").ap() if n_phases >= 5 else None

    QF = dscr("QF", [8, 65, S_TOK], BF16)
    KF = dscr("KF", [8, 65, S_TOK], BF16)
    QS = dscr("QS", [8, 64, S_TOK], BF16)
    KS = dscr("KS", [2, 64, S_TOK], BF16)
    AT = dscr("AT", [D, S_TOK], BF16)
    H2 = dscr("H2", [S_TOK, D], F32)
    XG = dscr("XG", [NE * CAP, D], BF16)
    YR = dscr("YR", [NE * CAP, D], F32)

    dbg = {"_declared": declared}

    def dout(name, shape, dt=F32):
        t = nc.dram_tensor(name, list(shape), dt, kind="ExternalOutput").ap()
        dbg[name] = t
        return t

    with contextlib.ExitStack() as st:
        S = Sched(nc, st)

        def mm(out_ap, lhsT, rhs, start=True, stop=True):
            return lambda e: e.matmul(out_ap, lhsT, rhs, start=start, stop=stop)

        def mmchain(lst):
            def fn(e):
                ins = None
                for (o, l, r, s0, s1) in lst:
                    ins = e.matmul(o, l, r, start=s0, stop=s1)
                return ins
            return fn

        def sbt(stack, name, shape, dt):
            return stack.enter_context(nc.sbuf_tensor(name, list(shape), dt))

        def pst(stack, name, shape, dt):
            return stack.enter_context(nc.psum_tensor(name, list(shape), dt))

        ident_f = sbt(st, "ident_f", [128, 128], F32)
        ident_b = sbt(st, "ident_b", [128, 128], BF16)
        mdiag = sbt(st, "mdiag", [128, 128], BF16)
        mprev = sbt(st, "mprev", [128, 128], BF16)
        ustrict = sbt(st, "ustrict", [128, 128], BF16)
        uincl_f = sbt(st, "uincl_f", [128, 128], F32)
        ones_b = sbt(st, "ones_b", [128, 128], BF16)
        ones_f = sbt(st, "ones_f", [128, 128], F32)
        sel127 = sbt(st, "sel127", [128, 128], F32)
        DEST = sbt(st, "DEST", [128, NT, 2], I32)
        GATE = sbt(st, "GATE", [128, NT, 2], F32)
        s012 = contextlib.ExitStack()
        VA = sbt(s012, "VA", [128, NT, 8, 65], BF16)
        VAs = sbt(s012, "VAs", [128, NT, 2, 65], BF16)
        Cc = sbt(s012, "Cc", [128, NT, 8], F32)
        CendB = sbt(s012, "CendB", [128, NT, 8], F32)
        ESINK = sbt(s012, "ESINK", [128, 8], F32)
        B_const = Buf("const")
        B_VA = Buf("VA")
        B_VAs = Buf("VAs")
        B_DEST = Buf("DEST")
        B_GATE = Buf("GATE")
        B_C = Buf("C")
        B_CendB = Buf("CendB")
        B_ES = Buf("ES")
        B_QF, B_KF, B_QS, B_KS, B_AT, B_H2, B_XG, B_YR = (Buf(n) for n in
                                                         ("QF", "KF", "QS", "KS", "AT", "H2", "XG", "YR"))

        def const_setup():
            w = [B_const]
            S.op("pool", lambda e: e.memset(ident_f[:], 1.0), writes=w)
            S.op("pool", lambda e: e.affine_select(ident_f[:], ident_f[:], pattern=[[-1, 128]],
                                                   compare_op=ALU.is_equal, fill=0.0, base=0,
                                                   channel_multiplier=1), reads=w, writes=w)
            S.op("pool", lambda e: e.memset(ident_b[:], 1.0), writes=w)
            S.op("pool", lambda e: e.affine_select(ident_b[:], ident_b[:], pattern=[[-1, 128]],
                                                   compare_op=ALU.is_equal, fill=0.0, base=0,
                                                   channel_multiplier=1), reads=w, writes=w)
            S.op("pool", lambda e: e.memset(mdiag[:], 1.0), writes=w)
            S.op("pool", lambda e: e.affine_select(mdiag[:], mdiag[:], pattern=[[1, 128]],
                                                   compare_op=ALU.is_ge, fill=0.0, base=0,
                                                   channel_multiplier=-1), reads=w, writes=w)
            S.op("pool", lambda e: e.memset(mprev[:], 1.0), writes=w)
            S.op("pool", lambda e: e.affine_select(mprev[:], mprev[:], pattern=[[-1, 128]],
                                                   compare_op=ALU.is_gt, fill=0.0, base=0,
                                                   channel_multiplier=1), reads=w, writes=w)
            S.op("pool", lambda e: e.memset(ustrict[:], 1.0), writes=w)
            S.op("pool", lambda e: e.affine_select(ustrict[:], ustrict[:], pattern=[[1, 128]],
                                                   compare_op=ALU.is_gt, fill=0.0, base=0,
                                                   channel_multiplier=-1), reads=w, writes=w)
            S.op("pool", lambda e: e.memset(uincl_f[:], 1.0), writes=w)
            S.op("pool", lambda e: e.affine_select(uincl_f[:], uincl_f[:], pattern=[[1, 128]],
                                                   compare_op=ALU.is_ge, fill=0.0, base=0,
                                                   channel_multiplier=-1), reads=w, writes=w)
            S.op("pool", lambda e: e.memset(ones_b[:], 1.0), writes=w)
            S.op("pool", lambda e: e.memset(ones_f[:], 1.0), writes=w)
            S.op("pool", lambda e: e.memset(sel127[:], 1.0), writes=w)
            S.op("pool", lambda e: e.affine_select(sel127[:], sel127[:], pattern=[[0, 128]],
                                                   compare_op=ALU.is_equal, fill=0.0, base=-127,
                                                   channel_multiplier=1), reads=w, writes=w)
            S.op("pool", lambda e: e.memset(VA[:, :, :, 64:65], 1.0), writes=[B_VA])
            S.op("pool", lambda e: e.memset(VAs[:, :, :, 64:65], 1.0), writes=[B_VAs])

        const_setup()

        def stop_here():
            dd_ = dout("d_done", [128, 128])
            S.dma("sp", lambda e: e.dma_start(out=dd_, in_=ident_f[:]), reads=[B_const])
            S.flush()
            return nc, dbg

        with contextlib.ExitStack() as s01:
            COS = sbt(s01, "COS", [128, S_TOK], F32)
            SINS = sbt(s01, "SINS", [128, S_TOK], F32)
            B_COS, B_SIN = Buf("COS"), Buf("SINS")
            wfm = sbt(s01, "wfm", [128, 8, 2304], BF16)
            wtm = sbt(s01, "wtm", [128, 8, 648], BF16)
            B_wfm, B_wtm = Buf(), Buf()
            wfm_v = w_fm.rearrange("(kc p) n -> p kc n", p=128)
            wtm_v = w_tm.rearrange("(kc p) n -> p kc n", p=128)
            for kc in range(8):
                S.dma("pool", lambda e, kc=kc: e.dma_start(out=wfm[:, kc, :], in_=wfm_v[:, kc, :]), writes=[B_wfm])
            S.dma("pool", lambda e: e.dma_start(out=wtm[:], in_=wtm_v), writes=[B_wtm])
            with contextlib.ExitStack() as s0:
                posi = sbt(s0, "posi", [128, S_TOK], I32)
                invf = sbt(s0, "invf", [128, 1], F32)
                sgn = sbt(s0, "sgn", [128, 1], F32)
                negpi = sbt(s0, "negpi", [128, 1], F32)
                B_pos, B_cf = Buf(), Buf()
                S.dma("sp", lambda e: e.dma_start(out=posi[:], in_=pos.partition_broadcast(128)), writes=[B_pos])
                S.dma("sp", lambda e: e.dma_start(out=invf[:], in_=cinv), writes=[B_cf])
                S.dma("sp", lambda e: e.dma_start(out=sgn[:], in_=csgn), writes=[B_cf])
                sk = sbt(s0, "sk", [128, 8], F32)
                B_sk = Buf()
                S.dma("sp", lambda e: e.dma_start(out=sk[:], in_=sinks.partition_broadcast(128)), writes=[B_sk])
                S.op("act", lambda e: e.activation(ESINK[:], sk[:], AF.Exp), reads=[B_sk], writes=[B_ES])
                CH = 1024
                tl = [(sbt(s0, f"rp_a{i}", [128, CH], F32), Buf()) for i in range(2)]
                tk = [(sbt(s0, f"rp_k{i}", [128, CH], I32), Buf()) for i in range(2)]
                tr = [(sbt(s0, f"rp_r{i}", [128, CH], F32), Buf()) for i in range(2)]
                for c in range(S_TOK // CH):
                    sl = slice(c * CH, (c + 1) * CH)
                    for which in range(2):
                        a, Ba = tl[which]
                        k, Bk = tk[which]
                        r, Br = tr[which]
                        S.op("dve", lambda e, a=a, sl=sl: e.tensor_copy(a[:], posi[:, sl]), reads=[B_pos], writes=[Ba])
                        S.op("dve", lambda e, a=a, which=which: e.tensor_scalar(
                            a[:], a[:], invf[:, 0:1], (np.pi / 2 if which else 0.0), op0=ALU.mult, op1=ALU.add),
                            reads=[Ba, B_cf], writes=[Ba])
                        S.op("dve", lambda e, a=a, r=r: e.tensor_scalar(r[:], a[:], 1.0 / TWO_PI, None, op0=ALU.mult),
                             reads=[Ba], writes=[Br])
                        S.op("dve", lambda e, k=k, r=r: e.tensor_copy(k[:], r[:]), reads=[Br], writes=[Bk])
                        S.op("dve", lambda e, k=k, r=r: e.tensor_copy(r[:], k[:]), reads=[Bk], writes=[Br])
                        S.op("dve", lambda e, a=a, r=r: e.scalar_tensor_tensor(
                            a[:], r[:], -TWO_PI, a[:], op0=ALU.mult, op1=ALU.add), reads=[Ba, Br], writes=[Ba])
                        S.op("dve", lambda e, a=a: e.tensor_scalar(a[:], a[:], 3.1415, -3.1415, op0=ALU.min, op1=ALU.max),
                             reads=[Ba], writes=[Ba])
                        if which == 0:
                            S.op("act", lambda e, a=a, sl=sl: e.activation(SINS[:, sl], a[:], AF.Sin),
                                 reads=[Ba], writes=[B_SIN])
                            S.op("dve", lambda e, sl=sl: e.tensor_scalar(SINS[:, sl], SINS[:, sl], sgn[:, 0:1], None,
                                                                         op0=ALU.mult),
                                 reads=[B_SIN, B_cf], writes=[B_SIN])
                        else:
                            S.op("act", lambda e, a=a, sl=sl: e.activation(COS[:, sl], a[:], AF.Sin),
                                 reads=[Ba], writes=[B_COS])
                S.flush()

            if debug and n_phases == 0:
                dcos = dout("d_cos", [128, S_TOK])
                dsin = dout("d_sin", [128, S_TOK])
                S.dma("sp", lambda e: e.dma_start(out=dcos, in_=COS[:]), reads=[B_COS])
                S.dma("sp", lambda e: e.dma_start(out=dsin, in_=SINS[:]), reads=[B_SIN])
                S.flush()
                return nc, dbg

            with contextlib.ExitStack() as s1:
                bfg = sbt(s1, "bfg", [128, 8], F32)
                B_bfg = Buf()
                S.dma("sp", lambda e: e.dma_start(out=bfg[:], in_=b_forget.partition_broadcast(128)), writes=[B_bfg])
                zt = sbt(s1, "zt", [128, 4096], BF16)
                B_zt = Buf()
                S.op("pool", lambda e: e.memset(zt[:], 0.0), writes=[B_zt])
                XGv = XG.rearrange("(a p r) d -> a p (r d)", p=128, r=4)
                for a in range(32):
                    S.dma("sp", lambda e, a=a: e.dma_start(out=XGv[a], in_=zt[:]), reads=[B_zt], writes=[B_XG])
                onesrow = sbt(s1, "onesrow", [1, S_TOK], BF16)
                B_or = Buf()
                S.op("pool", lambda e: e.memset(onesrow[:], 1.0), writes=[B_or])
                for h in range(8):
                    S.dma("sp", lambda e, h=h: e.dma_start(out=KF[h, 64:65, :], in_=onesrow[:]), reads=[B_or], writes=[B_KF])

                if STOP == "a":
                    return stop_here()
                xch = Rot([(sbt(s1, f"xch{i}", [128, 8, 512], BF16), Buf()) for i in range(2)])
                psf = Rot([(pst(s1, f"psf{i}", [128, 512], F32), Buf()) for i in range(4)])
                pta = Rot([(pst(s1, f"pta{i}", [128, 512], F32), Buf()) for i in range(1)])
                ptb = Rot([(pst(s1, f"ptb{i}", [128, 136], F32), Buf()) for i in range(1)])
                stg = Rot([(sbt(s1, f"stg{i}", [128, 512], BF16), Buf()) for i in range(3)])
                t1r = Rot([(sbt(s1, f"t1_{i}", [128, 512], F32), Buf()) for i in range(2)])
                t2r = Rot([(sbt(s1, f"t2_{i}", [128, 512], F32), Buf()) for i in range(2)])
                LF = sbt(s1, "LF", [128, NT, 8], F32)
                B_LF = Buf()
                pbs_r = Rot([(sbt(s1, f"pbs{i}", [128, 136], F32), Buf()) for i in range(2)])
                xT_v = xT.rearrange("(kc p) t -> p kc t", p=128)

                def evac_store(ps, Bps, dst_list, idx, engine):
                    sg, Bsg = stg.next()
                    if engine == "act":
                        S.op("act", lambda e: e.copy(sg[:], ps[:]), reads=[Bps], writes=[Bsg])
                    else:
                        S.op("dve", lambda e: e.tensor_copy(sg[:], ps[:]), reads=[Bps], writes=[Bsg])
                    return sg, Bsg

                for tc in range(8 if STOP not in ("b", "b1", "b2", "b1m", "b1p", "t1", "t2") else 1):
                    t0 = tc * 512
                    xc, Bxc = xch.next()
                    S.dma("pool", lambda e, xc=xc, t0=t0: e.dma_start(out=xc[:], in_=xT_v[:, :, t0:t0 + 512]), writes=[Bxc])

                    def fm_chunk(mc, xc=xc, Bxc=Bxc):
                        ps, Bps = psf.next()
                        S.op("pe", mmchain([(ps[:], wfm[:, kc, mc * 128:(mc + 1) * 128], xc[:, kc, :], kc == 0, kc == 7)
                                            for kc in range(8)]), reads=[B_wfm, Bxc], writes=[Bps])
                        return ps, Bps

                    if STOP == "b0":
                        return stop_here()
                    for mc in range(8):
                        ps, Bps = fm_chunk(mc)
                        if STOP == "b1p":
                            continue
                        sg, Bsg = evac_store(ps, Bps, None, None, "act" if mc % 2 == 0 else "dve")
                        dst, Bd = (QF, B_QF) if mc < 4 else (KF, B_KF)
                        for j in range(2 if STOP != "b1m" else 0):
                            h = (mc % 4) * 2 + j
                            S.dma("sp", lambda e, dst=dst, h=h, j=j, sg=sg, t0=t0: e.dma_start(
                                out=dst[h, 0:64, t0:t0 + 512], in_=sg[j * 64:(j + 1) * 64, :]), reads=[Bsg], writes=[Bd])
                    for c in range(5 if STOP not in ("b1", "b1m", "b1p") else 0):
                        mc_a = 8 + c if c < 4 else 16
                        mc_b = 12 + c if c < 4 else 17
                        psa, Bpa = fm_chunk(mc_a)
                        psb, Bpb = fm_chunk(mc_b)
                        t1, Bt1 = t1r.next()
                        t2, Bt2 = t2r.next()
                        S.op("dve", lambda e, t1=t1, psa=psa, t0=t0: e.tensor_tensor(t1[:], psa[:], COS[:, t0:t0 + 512], ALU.mult),
                             reads=[Bpa, B_COS], writes=[Bt1])
                        S.op("dve", lambda e, t2=t2, psb=psb, t0=t0: e.tensor_tensor(t2[:], psb[:], SINS[:, t0:t0 + 512], ALU.mult),
                             reads=[Bpb, B_SIN], writes=[Bt2])
                        sg, Bsg = stg.next()
                        S.op("pool", lambda e, sg=sg, t1=t1, t2=t2: e.tensor_tensor(sg[:], t1[:], t2[:], ALU.add),
                             reads=[Bt1, Bt2], writes=[Bsg])
                        for j in range(2):
                            if c < 4:
                                dst, Bd, h = QS, B_QS, c * 2 + j
                            else:
                                dst, Bd, h = KS, B_KS, j
                            S.dma("sp", lambda e, dst=dst, h=h, j=j, sg=sg, t0=t0: e.dma_start(
                                out=dst[h, 0:64, t0:t0 + 512], in_=sg[j * 64:(j + 1) * 64, :]), reads=[Bsg], writes=[Bd])
                    for tt in range(4 if STOP not in ("b1", "b2", "b1m", "b1p") else 0):
                        ti = tc * 4 + tt
                        pa, Bpa = pta.next()
                        pb, Bpb = ptb.next()
                        S.op("pe", mmchain([(pa[:], xc[:, kc, tt * 128:(tt + 1) * 128], wtm[:, kc, 0:512], kc == 0, kc == 7)
                                            for kc in range(8)]), reads=[B_wtm, Bxc], writes=[Bpa])
                        S.op("act", lambda e, pa=pa, ti=ti: e.copy(VA[:, ti, :, 0:64], pa[:].rearrange("p (h d) -> p h d", h=8)),
                             reads=[Bpa], writes=[B_VA])
                        if STOP == "t1":
                            continue
                        S.op("pe", mmchain([(pb[:], xc[:, kc, tt * 128:(tt + 1) * 128], wtm[:, kc, 512:648], kc == 0, kc == 7)
                                            for kc in range(8)]), reads=[B_wtm, Bxc], writes=[Bpb])
                        pbs, Bpbs = pbs_r.next()
                        S.op("act", lambda e, pb=pb, pbs=pbs: e.copy(pbs[:], pb[:]), reads=[Bpb], writes=[Bpbs])
                        S.op("pool", lambda e, pbs=pbs, ti=ti: e.tensor_copy(VAs[:, ti, :, 0:64],
                                                                           pbs[:, 0:128].rearrange("p (h d) -> p h d", h=2)),
                             reads=[Bpbs], writes=[B_VAs])
                        if STOP == "t2":
                            continue
                        S.op("pool", lambda e, pbs=pbs, ti=ti: e.tensor_tensor(LF[:, ti, :], pbs[:, 128:136], bfg[:], ALU.add),
                             reads=[Bpbs, B_bfg], writes=[B_LF])

                if STOP in ("b", "c", "b1", "b2", "b1m", "b1p", "t1", "t2"):
                    return stop_here()
                LF2 = LF[:].rearrange("p t h -> p (t h)")
                S.op("act", lambda e: e.activation(LF2, LF2, AF.Exp, scale=-1.0), reads=[B_LF], writes=[B_LF])
                S.op("act", lambda e: e.activation(LF2, LF2, AF.Ln, bias=ones_f[:, 0:1]), reads=[B_LF, B_const], writes=[B_LF])
                S.op("dve", lambda e: e.tensor_scalar(LF2, LF2, -1.0, None, op0=ALU.mult), reads=[B_LF], writes=[B_LF])
                pc1, Bpc1 = psf.next()
                pc2, Bpc2 = psf.next()
                S.op("pe", mm(pc1[:, 0:256], uincl_f[:], LF2), reads=[B_LF, B_const], writes=[Bpc1])
                S.op("pe", mm(pc2[:, 0:256], ones_f[:], LF2), reads=[B_LF, B_const], writes=[Bpc2])
                TOT = sbt(s1, "TOT", [128, NT, 8], F32)
                CAR = sbt(s1, "CAR", [128, NT, 8], F32)
                B_TOT, B_CAR = Buf(), Buf()
                S.op("act", lambda e: e.copy(TOT[:].rearrange("p t h -> p (t h)"), pc2[:, 0:256]), reads=[Bpc2], writes=[B_TOT])
                S.op("dve", lambda e: e.memset(CAR[:, 0, :], 0.0), writes=[B_CAR])
                for i in range(1, NT):
                    S.op("dve", lambda e, i=i: e.tensor_tensor(CAR[:, i, :], CAR[:, i - 1, :], TOT[:, i - 1, :], ALU.add),
                         reads=[B_TOT, B_CAR], writes=[B_CAR])
                S.op("dve", lambda e: e.tensor_tensor(Cc[:].rearrange("p t h -> p (t h)"), pc1[:, 0:256],
                                                      CAR[:].rearrange("p t h -> p (t h)"), ALU.add),
                     reads=[Bpc1, B_CAR], writes=[B_C])
                if STOP == "d1":
                    dC = dout("d_C", [128, NT * 8])
                    S.dma("sp", lambda e: e.dma_start(out=dC, in_=Cc[:].rearrange("p t h -> p (t h)")), reads=[B_C])
                    return stop_here()
                pc3, Bpc3 = psf.next()
                S.op("pe", mm(pc3[:, 0:256], sel127[:], Cc[:].rearrange("p t h -> p (t h)")), reads=[B_C, B_const], writes=[Bpc3])
                S.op("act", lambda e: e.copy(CendB[:].rearrange("p t h -> p (t h)"), pc3[:, 0:256]), reads=[Bpc3], writes=[B_CendB])
                if STOP == "d2":
                    dC = dout("d_C", [128, NT * 8])
                    S.dma("sp", lambda e: e.dma_start(out=dC, in_=CendB[:].rearrange("p t h -> p (t h)")), reads=[B_CendB])
                    return stop_here()
                rsm = sbt(s1, "rsm", [1, 8, NT], F32)
                B_rsm = Buf()
                for h in range(8):
                    S.op("dve", lambda e, h=h: e.tensor_tensor(
                        rsm[0:1, h, :].rearrange("p (a b) -> p a b", b=4),
                        CendB[0:1, :, h].rearrange("p (a b) -> p a b", b=4),
                        CendB[0:1, :, h].rearrange("p (a b) -> p a b", b=4)[:, :, 3:4].to_broadcast([1, 8, 4]),
                        ALU.subtract), reads=[B_CendB], writes=[B_rsm])
                if STOP == "d3":
                    return stop_here()
                rrow = Rot([(sbt(s1, f"rrow{i}", [1, NT, 128], BF16), Buf()) for i in range(2)])
                for h in range(8):
                    rr, Brr = rrow.next()
                    S.op("dve", lambda e, h=h, rr=rr: e.tensor_scalar(
                        rr[:], rsm[0:1, h, :].unsqueeze(2).to_broadcast([1, NT, 128]), 8.0, None, op0=ALU.mult),
                        reads=[B_rsm], writes=[Brr])
                    S.dma("sp", lambda e, h=h, rr=rr: e.dma_start(out=QF[h, 64:65, :], in_=rr[:].rearrange("p a b -> p (a b)")),
                          reads=[Brr], writes=[B_QF])
                if debug and n_phases == 1:
                    dC = dout("d_C", [128, NT * 8])
                    dVA = dout("d_VA", [128, NT * 8 * 65], BF16)
                    S.dma("sp", lambda e: e.dma_start(out=dC, in_=Cc[:].rearrange("p t h -> p (t h)")), reads=[B_C])
                    S.dma("sp", lambda e: e.dma_start(out=dVA, in_=VA[:].rearrange("p t h d -> p (t h d)")), reads=[B_VA])
                    for nm, src, Bs, shp in (("d_QF", QF, B_QF, [8, 65, S_TOK]), ("d_KF", KF, B_KF, [8, 65, S_TOK]),
                                             ("d_QS", QS, B_QS, [8, 64, S_TOK]), ("d_KS", KS, B_KS, [2, 64, S_TOK])):
                        dd = dout(nm, shp, BF16)
                        S.dma("sp", lambda e, dd=dd, src=src: e.dma_start(out=dd, in_=src), reads=[Bs])
                S.flush()
        if n_phases <= 1:
            return nc, dbg

        with contextlib.ExitStack() as s2:
            BIAS = sbt(s2, "BIAS", [128, 8, NT, 8], F32)
            B_BIAS = Buf()
            Cend512 = CendB[:].rearrange("p (a b) h -> p a b h", b=4)
            for h in range(8):
                for kt in range(NT):
                    S.op("dve", lambda e, h=h, kt=kt: e.tensor_scalar(
                        BIAS[:, h, kt, :], Cend512[:, :, 3, h], Cc[:, kt, h:h + 1], None, op0=ALU.subtract),
                        reads=[B_CendB, B_C], writes=[B_BIAS])
            qld = Rot([(sbt(s2, f"qld{i}", [65, S_TOK], BF16), Buf()) for i in range(2)])
            kld = Rot([(sbt(s2, f"kld{i}", [65, S_TOK], BF16), Buf()) for i in range(2)])
            pss = Rot([(pst(s2, f"pss{i}", [128, 512], F32), Buf()) for i in range(4)])
            pso = Rot([(pst(s2, f"pso{i}", [65, 512], F32), Buf()) for i in range(2)])
            psb = Rot([(pst(s2, f"psb{i}", [64, 512], F32), Buf()) for i in range(1)])
            ptr = Rot([(sbt(s2, f"pT{i}", [128, 512], BF16), Buf()) for i in range(6)])
            ost = Rot([(sbt(s2, f"ost{i}", [64, S_TOK], BF16), Buf()) for i in range(2)])
            rec = Rot([(sbt(s2, f"rec{i}", [65, 512], F32), Buf()) for i in range(3)])
            bcs = Rot([(sbt(s2, f"bcs{i}", [64, 512], F32), Buf()) for i in range(2)])

            def normalize(po, Bpo, os_, Bos, QB, sink_h=None):
                rc, Brc = rec.next()
                if sink_h is None:
                    S.op("dve", lambda e: e.reciprocal(rc[64:65, :], po[64:65, :]), reads=[Bpo], writes=[Brc])
                else:
                    S.op("dve", lambda e: e.tensor_scalar(rc[64:65, :], po[64:65, :], ESINK[64:65, sink_h:sink_h + 1], None,
                                                          op0=ALU.add), reads=[Bpo, B_ES], writes=[Brc])
                    S.op("dve", lambda e: e.reciprocal(rc[64:65, :], rc[64:65, :]), reads=[Brc], writes=[Brc])
                pb, Bpb = psb.next()
                S.op("pe", mm(pb[:], ones_f[64:65, 0:64], rc[64:65, :]), reads=[Brc, B_const], writes=[Bpb])
                bc, Bbc = bcs.next()
                S.op("act", lambda e: e.copy(bc[:], pb[:]), reads=[Bpb], writes=[Bbc])
                S.op("dve", lambda e: e.tensor_tensor(os_[:, QB * 512:(QB + 1) * 512], po[0:64, :], bc[:], ALU.mult),
                     reads=[Bpo, Bbc], writes=[Bos])

            LA = 3
            qk_bufs = {}

            def load_fox(h):
                q, Bq = qld.next()
                k, Bk = kld.next()
                S.dma("sp", lambda e: e.dma_start(out=q[:], in_=QF[h]), reads=[B_QF], writes=[Bq])
                S.dma("sp", lambda e: e.dma_start(out=k[:], in_=KF[h]), reads=[B_KF], writes=[Bk])
                qk_bufs[("f", h)] = (q, Bq, k, Bk)

            def load_swa(h):
                g = h // 4
                q, Bq = qld.next()
                k, Bk = kld.next()
                S.dma("sp", lambda e: e.dma_start(out=q[0:64, :], in_=QS[h]), reads=[B_QS], writes=[Bq])
                S.dma("sp", lambda e: e.dma_start(out=k[0:64, :], in_=KS[g]), reads=[B_KS], writes=[Bk])
                qk_bufs[("s", h)] = (q, Bq, k, Bk)

            load_fox(0)
            pending_norm = []
            for h in range(8):
                q, Bq, k, Bk = qk_bufs.pop(("f", h))
                if h + 1 < 8:
                    load_fox(h + 1)
                else:
                    load_swa(0)
                os_, Bos = ost.next()
                tiles = [(QB, kt) for QB in range(8) for kt in range(4 * QB + 4)]
                n = len(tiles)
                pend = {}

                def emit_qk(i, q=q, k=k, Bq=Bq, Bk=Bk):
                    QB, kt = tiles[i]
                    j = kt - 4 * QB
                    c0 = 128 * j if j > 0 else 0
                    ps, Bps = pss.next()
                    S.op("pe", mm(ps[:, c0:512], k[:, kt * 128:(kt + 1) * 128], q[:, QB * 512 + c0:(QB + 1) * 512]),
                         reads=[Bq, Bk], writes=[Bps])
                    pend[i] = (ps, Bps, c0, j)

                for i in range(min(LA, n)):
                    emit_qk(i)
                po = Bpo = None
                for i, (QB, kt) in enumerate(tiles):
                    nkt = 4 * QB + 4
                    if kt == 0:
                        po, Bpo = pso.next()
                    ps, Bps, c0, j = pend.pop(i)
                    pT, BpT = ptr.next()
                    S.op("act", lambda e, pT=pT, ps=ps, c0=c0, h=h, kt=kt, QB=QB: e.activation(
                        pT[:, c0:512], ps[:, c0:512], AF.Exp, bias=BIAS[:, h, kt, QB:QB + 1], scale=0.125),
                        reads=[Bps, B_BIAS], writes=[BpT])
                    if j >= 0:
                        S.op("pool", lambda e, pT=pT, c0=c0: e.tensor_tensor(pT[:, c0:c0 + 128], pT[:, c0:c0 + 128],
                                                                               mdiag[:], ALU.mult),
                             reads=[BpT, B_const], writes=[BpT])
                    if i + LA < n:
                        emit_qk(i + LA)
                    S.op("pe", mm(po[:, c0:512], VA[:, kt, h, :], pT[:, c0:512], start=(kt == 0), stop=(kt == nkt - 1)),
                         reads=[BpT, B_VA], writes=[Bpo])
                    if kt == 2 and pending_norm:
                        pending_norm.pop(0)()
                    if kt == nkt - 1:
                        pending_norm.append(lambda po=po, Bpo=Bpo, os_=os_, Bos=Bos, QB=QB: normalize(po, Bpo, os_, Bos, QB))
                while pending_norm:
                    pending_norm.pop(0)()
                S.dma("sp", lambda e, h=h, os_=os_: e.dma_start(out=AT[h * 64:(h + 1) * 64, :], in_=os_[:]),
                      reads=[Bos], writes=[B_AT])

            units = [(h, QB) for h in range(8) for QB in range(8)]
            upend = {}
            hstate = {}

            def swa_qk(u):
                h, QB = units[u]
                if QB == 0:
                    q, Bq, k, Bk = qk_bufs.pop(("s", h))
                    os_, Bos = ost.next()
                    hstate[h] = (q, Bq, k, Bk, os_, Bos)
                    if h + 1 < 8:
                        load_swa(h + 1)
                q, Bq, k, Bk, os_, Bos = hstate[h]
                psA, BpsA = pss.next()
                psB, BpsB = pss.next()
                lstA, lstB = [], []
                for j in range(4):
                    qb = 4 * QB + j
                    qs = q[0:64, qb * 128:(qb + 1) * 128]
                    if qb > 0:
                        lstA.append((psA[:, j * 128:(j + 1) * 128], k[0:64, (qb - 1) * 128:qb * 128], qs, True, True))
                    lstB.append((psB[:, j * 128:(j + 1) * 128], k[0:64, qb * 128:(qb + 1) * 128], qs, True, True))
                S.op("pe", mmchain(lstA), reads=[Bq, Bk], writes=[BpsA])
                S.op("pe", mmchain(lstB), reads=[Bq, Bk], writes=[BpsB])
                upend[u] = (psA, BpsA, psB, BpsB)

            swa_qk(0)
            for u, (h, QB) in enumerate(units):
                g = h // 4
                q, Bq, k, Bk, os_, Bos = hstate[h]
                psA, BpsA, psB, BpsB = upend.pop(u)
                po, Bpo = pso.next()
                if pending_norm:
                    pending_norm.pop(0)()
                jA0 = 1 if QB == 0 else 0
                pA, BpA = ptr.next()
                pB, BpB = ptr.next()
                S.op("act", lambda e, pA=pA, psA=psA, jA0=jA0: e.activation(pA[:, jA0 * 128:512], psA[:, jA0 * 128:512],
                                                                          AF.Exp, scale=0.125),
                     reads=[BpsA], writes=[BpA])
                S.op("act", lambda e, pB=pB, psB=psB: e.activation(pB[:], psB[:], AF.Exp, scale=0.125),
                     reads=[BpsB], writes=[BpB])
                S.op("pool", lambda e, pA=pA, jA0=jA0: e.tensor_tensor(
                    pA[:, jA0 * 128:512].rearrange("p (a b) -> p a b", b=128), pA[:, jA0 * 128:512].rearrange("p (a b) -> p a b", b=128),
                    mprev[:].unsqueeze(1).to_broadcast([128, 4 - jA0, 128]), ALU.mult), reads=[BpA, B_const], writes=[BpA])
                S.op("dve", lambda e, pB=pB: e.tensor_tensor(
                    pB[:].rearrange("p (a b) -> p a b", b=128), pB[:].rearrange("p (a b) -> p a b", b=128),
                    mdiag[:].unsqueeze(1).to_broadcast([128, 4, 128]), ALU.mult), reads=[BpB, B_const], writes=[BpB])
                if u + 1 < len(units):
                    swa_qk(u + 1)
                lst = []
                for j in range(4):
                    qb = 4 * QB + j
                    cs = slice(j * 128, (j + 1) * 128)
                    if qb > 0:
                        lst.append((po[:, cs], VAs[:, qb - 1, g, :], pA[:, cs], True, False))
                        lst.append((po[:, cs], VAs[:, qb, g, :], pB[:, cs], False, True))
                    else:
                        lst.append((po[:, cs], VAs[:, qb, g, :], pB[:, cs], True, True))
                S.op("pe", mmchain(lst), reads=[BpA, BpB, B_VAs], writes=[Bpo])
                pending_norm.append(lambda po=po, Bpo=Bpo, os_=os_, Bos=Bos, QB=QB, h=h: normalize(po, Bpo, os_, Bos, QB, sink_h=h))
                if QB == 7:
                    while pending_norm:
                        pending_norm.pop(0)()
                    S.dma("sp", lambda e, h=h, os_=os_: e.dma_start(out=AT[512 + h * 64:512 + (h + 1) * 64, :], in_=os_[:]),
                          reads=[Bos], writes=[B_AT])
            if debug and n_phases == 2:
                dd = dout("d_AT", [D, S_TOK], BF16)
                S.dma("sp", lambda e: e.dma_start(out=dd, in_=AT), reads=[B_AT])
            S.flush()
        s012.close()
        if n_phases <= 2:
            return nc, dbg

        with contextlib.ExitStack() as s3:
            wmo = sbt(s3, "wmo", [128, 8, D], BF16)
            wxq = sbt(s3, "wxq", [128, 8, D], BF16)
            wxo = sbt(s3, "wxo", [128, 8, D], BF16)
            KxT = sbt(s3, "KxT", [128, 8, 256], BF16)
            Vx = sbt(s3, "Vx", [128, 2, D], BF16)
            LNB = sbt(s3, "LNB", [128, 4, D], F32)
            wr = sbt(s3, "wr", [128, 8, 36], F32)
            brb = sbt(s3, "brb", [128, 36], F32)
            ebase = sbt(s3, "ebase", [128, NE], F32)
            cnt = sbt(s3, "cnt", [128, NE], F32)
            tokid = sbt(s3, "tokid", [128, 1], I32)
            B_w3, B_Kx, B_Vx, B_LNB, B_wr, B_cnt = Buf(), Buf(), Buf(), Buf(), Buf(), Buf()
            for (dst, src) in ((wmo, w_mix_out), (wxq, w_xq), (wxo, w_xout)):
                S.dma("pool", lambda e, dst=dst, src=src: e.dma_start(out=dst[:], in_=src.rearrange("(kc p) n -> p kc n", p=128)),
                      writes=[B_w3])
            for i in range(4):
                S.dma("sp", lambda e, i=i: e.dma_start(out=LNB[:, i, :], in_=lnp[i:i + 1, :].partition_broadcast(128)), writes=[B_LNB])
            S.dma("sp", lambda e: e.dma_start(out=wr[:], in_=w_r.rearrange("(kc p) n -> p kc n", p=128)), writes=[B_wr])
            S.dma("sp", lambda e: e.dma_start(out=brb[:], in_=b_r.partition_broadcast(128)), writes=[B_wr])
            S.op("pool", lambda e: e.iota(ebase[:], pattern=[[CAP, NE]], base=0, channel_multiplier=0,
                                          allow_small_or_imprecise_dtypes=True), writes=[B_wr])
            S.op("pool", lambda e: e.memset(cnt[:], 0.0), writes=[B_cnt])
            with contextlib.ExitStack() as s3a:
                wkv = sbt(s3a, "wkv", [128, 8, 2 * D], BF16)
                memb = sbt(s3a, "memb", [128, 8, 256], BF16)
                B_wkv, B_memb = Buf(), Buf()
                for kc in range(8):
                    S.dma("pool", lambda e, kc=kc: e.dma_start(out=wkv[:, kc, :], in_=w_xkv.rearrange("(kc p) n -> p kc n", p=128)[:, kc, :]),
                          writes=[B_wkv])
                S.dma("pool", lambda e: e.dma_start(out=memb[:], in_=memT.rearrange("(kc p) n -> p kc n", p=128)), writes=[B_memb])
                pk = Rot([(pst(s3a, f"pk{i}", [128, 512], F32), Buf()) for i in range(2)])
                for cc in range(8):
                    p_, Bp_ = pk.next()
                    S.op("pe", mmchain([(p_[:, 0:256], wkv[:, kc, cc * 128:(cc + 1) * 128], memb[:, kc, :], kc == 0, kc == 7)
                                        for kc in range(8)]), reads=[B_wkv, B_memb], writes=[Bp_])
                    S.op("act", lambda e, p_=p_, cc=cc: e.copy(KxT[:, cc, :], p_[:, 0:256]), reads=[Bp_], writes=[B_Kx])
                for mt in range(2):
                    for half in range(2):
                        p_, Bp_ = pk.next()
                        S.op("pe", mmchain([(p_[:], memb[:, kc, mt * 128:(mt + 1) * 128],
                                             wkv[:, kc, D + half * 512:D + (half + 1) * 512], kc == 0, kc == 7)
                                            for kc in range(8)]), reads=[B_wkv, B_memb], writes=[Bp_])
                        S.op("dve", lambda e, p_=p_, mt=mt, half=half: e.tensor_copy(Vx[:, mt, half * 512:(half + 1) * 512], p_[:]),
                             reads=[Bp_], writes=[B_Vx])
                S.flush()

            tpb = pst(s3, "tpb", [128, D], BF16)
            B_tpb = Buf()
            stp_t = pst(s3, "stp", [128, 2, 2, 128], F32)
            stp = Rot([(stp_t[:, i], Buf()) for i in range(2)])
            pvp = Rot([(pst(s3, f"pvp{i}", [128, 4, 128], F32), Buf()) for i in range(1)])
            rtp = pst(s3, "rtp", [128, 256], F32)
            B_rtp = Buf()
            at_r = Rot([(sbt(s3, f"att{i}", [128, 8, 128], BF16), Buf()) for i in range(2)])
            xt_r = Rot([(sbt(s3, f"xtk{i}", [128, D], F32), Buf()) for i in range(2)])
            sqj = sbt(s3, "sqj", [128, D], F32)
            B_sqj = Buf()
            pTx = Rot([(sbt(s3, f"pTx{i}", [128, 2, 128], BF16), Buf()) for i in range(2)])
            recx = Rot([(sbt(s3, f"recx{i}", [128, 128], F32), Buf()) for i in range(2)])
            h2_r = Rot([(sbt(s3, f"h2_{i}", [128, D], F32), Buf()) for i in range(2)])
            h2b_r = Rot([(sbt(s3, f"h2b_{i}", [128, D], BF16), Buf()) for i in range(2)])

            class Cx:
                pass

            cxs = []
            for si in range(2):
                cx = Cx()
                cx.big = pst(s3, f"big{si}", [128, D], F32)
                cx.pre = sbt(s3, f"pre{si}", [128, D], F32)
                cx.xcn = sbt(s3, f"xcn{si}", [128, D], F32)
                cx.h1 = sbt(s3, f"h1_{si}", [128, D], F32)
                cx.h1b = sbt(s3, f"h1b{si}", [128, D], BF16)
                cx.h1T = sbt(s3, f"h1T{si}", [128, 8, 128], BF16)
                cx.qxT = sbt(s3, f"qxT{si}", [128, 8, 128], BF16)
                cx.oxT = sbt(s3, f"oxT{si}", [128, 8, 128], BF16)
                cx.h2T = sbt(s3, f"h2T{si}", [128, 8, 128], F32)
                cx.sm = sbt(s3, f"sm{si}", [128, 16], F32)
                cx.rt = sbt(s3, f"rt{si}", [128, 256], F32)
                cx.Mb = sbt(s3, f"Mb{si}", [128, 64], BF16)
                for nm in ("big", "pre", "xcn", "h1", "h1b", "h1T", "qxT", "oxT", "h2T", "sm", "rt", "Mb"):
                    setattr(cx, "B_" + nm, Buf())
                cxs.append(cx)

            class Indir:
                def __init__(self):
                    self.rec = None

                def op(self, *a_, **k_):
                    if self.rec is not None:
                        self.rec.append(("op", a_, k_))
                    else:
                        S.op(*a_, **k_)

                def dma(self, *a_, **k_):
                    if self.rec is not None:
                        self.rec.append(("dma", a_, k_))
                    else:
                        S.dma(*a_, **k_)

            SS = Indir()

            def layer_norm(cx, src_ap, Bsrc, gi, dst, Bdst):
                sm, B_sm, xcn, B_xcn = cx.sm, cx.B_sm, cx.xcn, cx.B_xcn
                SS.op("dve", lambda e: e.reduce_sum(sm[:, 0:1], src_ap, axis=AX.X), reads=[Bsrc], writes=[B_sm])
                SS.op("dve", lambda e: e.tensor_scalar(sm[:, 1:2], sm[:, 0:1], -1.0 / D, None, op0=ALU.mult),
                     reads=[B_sm], writes=[B_sm])
                SS.op("act", lambda e: e.activation(xcn[:], src_ap, AF.Identity, bias=sm[:, 1:2]),
                     reads=[Bsrc, B_sm], writes=[B_xcn])
                SS.op("act", lambda e: e.activation(sqj[:], xcn[:], AF.Square, accum_out=sm[:, 2:3]),
                     reads=[B_xcn], writes=[B_sqj, B_sm])
                SS.op("dve", lambda e: e.tensor_scalar(sm[:, 3:4], sm[:, 2:3], 1.0 / D, EPS, op0=ALU.mult, op1=ALU.add),
                     reads=[B_sm], writes=[B_sm])
                SS.op("act", lambda e: e.activation(sm[:, 4:5], sm[:, 3:4], AF.Sqrt), reads=[B_sm], writes=[B_sm])
                SS.op("dve", lambda e: e.reciprocal(sm[:, 5:6], sm[:, 4:5]), reads=[B_sm], writes=[B_sm])
                SS.op("dve", lambda e: e.scalar_tensor_tensor(xcn[:], xcn[:], sm[:, 5:6], LNB[:, gi, :], op0=ALU.mult, op1=ALU.mult),
                     reads=[B_xcn, B_sm, B_LNB], writes=[B_xcn])
                SS.op("pool", lambda e: e.tensor_tensor(dst[:], xcn[:], LNB[:, gi + 1, :], ALU.add),
                     reads=[B_xcn, B_LNB], writes=[Bdst])

            AT_v = AT.rearrange("(kc p) t -> p kc t", p=128)

            def st1(cx, ti):
                tsl = slice(ti * 128, (ti + 1) * 128)
                big, B_big, pre, B_pre = cx.big, cx.B_big, cx.pre, cx.B_pre
                att, Batt = at_r.next()
                xtk, Bxtk = xt_r.next()
                SS.dma("sp", lambda e: e.dma_start(out=att[:], in_=AT_v[:, :, tsl]), reads=[B_AT], writes=[Batt])
                SS.dma("sp", lambda e: e.dma_start(out=xtk[:], in_=x_tok[tsl, :]), writes=[Bxtk])
                SS.op("pe", mmchain([(big[:, hf * 512:(hf + 1) * 512], att[:, kc, :], wmo[:, kc, hf * 512:(hf + 1) * 512], kc == 0, kc == 7)
                                    for hf in range(2) for kc in range(8)]), reads=[Batt, B_w3], writes=[B_big])
                SS.op("dve", lambda e: e.scalar_tensor_tensor(pre[:], xtk[:], ALPHA, big[:], op0=ALU.mult, op1=ALU.add),
                     reads=[Bxtk, B_big], writes=[B_pre])
                layer_norm(cx, pre[:], B_pre, 0, cx.h1, cx.B_h1)

            def st2(cx, ti):
                big, B_big, h1, B_h1, h1b, B_h1b, h1T, B_h1T, qxT, B_qxT = (cx.big, cx.B_big, cx.h1, cx.B_h1, cx.h1b, cx.B_h1b,
                                                                           cx.h1T, cx.B_h1T, cx.qxT, cx.B_qxT)
                SS.op("act", lambda e: e.copy(h1b[:], h1[:]), reads=[B_h1], writes=[B_h1b])
                SS.op("pe", lambda e: [e.transpose(tpb[:, kc * 128:(kc + 1) * 128], h1b[:, kc * 128:(kc + 1) * 128], ident_b[:])
                                      for kc in range(8)][-1], reads=[B_h1b, B_const], writes=[B_tpb])
                SS.op("dve", lambda e: e.tensor_copy(h1T[:].rearrange("p a b -> p (a b)"), tpb[:]), reads=[B_tpb], writes=[B_h1T])
                SS.op("pe", mmchain([(big[:, cc * 128:(cc + 1) * 128], wxq[:, kc, cc * 128:(cc + 1) * 128], h1T[:, kc, :], kc == 0, kc == 7)
                                    for cc in range(8) for kc in range(8)]), reads=[B_h1T, B_w3], writes=[B_big])
                SS.op("act", lambda e: e.copy(qxT[:].rearrange("p a b -> p (a b)"), big[:]), reads=[B_big], writes=[B_qxT])

            def st3(cx, ti):
                qxT, B_qxT, oxT, B_oxT = cx.qxT, cx.B_qxT, cx.oxT, cx.B_oxT
                for hh in range(4):
                    sp_, Bsp = stp.next()
                    SS.op("pe", mmchain([(sp_[:, mt, :], KxT[:, 2 * hh + c, mt * 128:(mt + 1) * 128], qxT[:, 2 * hh + c, :], c == 0, c == 1)
                                        for mt in range(2) for c in range(2)]), reads=[B_Kx, B_qxT], writes=[Bsp])
                    pt_, Bpt = pTx.next()
                    SS.op("act", lambda e, pt_=pt_, sp_=sp_: e.activation(pt_[:], sp_[:], AF.Exp, scale=1.0 / 16.0),
                         reads=[Bsp], writes=[Bpt])
                    pv, Bpv = pvp.next()
                    lst = [(pv[:, 0, :], ones_b[:], pt_[:, mt, :], mt == 0, mt == 1) for mt in range(2)]
                    for c in range(2):
                        lst += [(pv[:, 1 + c, :], Vx[:, mt, (2 * hh + c) * 128:(2 * hh + c + 1) * 128], pt_[:, mt, :], mt == 0, mt == 1)
                                for mt in range(2)]
                    SS.op("pe", mmchain(lst), reads=[Bpt, B_Vx, B_const], writes=[Bpv])
                    rx, Brx = recx.next()
                    SS.op("dve", lambda e, rx=rx, pv=pv: e.reciprocal(rx[:], pv[:, 0, :]), reads=[Bpv], writes=[Brx])
                    SS.op("dve", lambda e, rx=rx, pv=pv, hh=hh: e.tensor_tensor(
                        oxT[:, 2 * hh:2 * hh + 2, :], pv[:, 1:3, :], rx[:].unsqueeze(1).to_broadcast([128, 2, 128]), ALU.mult),
                        reads=[Bpv, Brx], writes=[B_oxT])

            def st4(cx, ti):
                tsl = slice(ti * 128, (ti + 1) * 128)
                big, B_big, pre, B_pre, h1, B_h1, oxT, B_oxT = cx.big, cx.B_big, cx.pre, cx.B_pre, cx.h1, cx.B_h1, cx.oxT, cx.B_oxT
                SS.op("pe", mmchain([(big[:, hf * 512:(hf + 1) * 512], oxT[:, kc, :], wxo[:, kc, hf * 512:(hf + 1) * 512], kc == 0, kc == 7)
                                    for hf in range(2) for kc in range(8)]), reads=[B_oxT, B_w3], writes=[B_big])
                SS.op("dve", lambda e: e.scalar_tensor_tensor(pre[:], h1[:], ALPHA, big[:], op0=ALU.mult, op1=ALU.add),
                     reads=[B_h1, B_big], writes=[B_pre])
                h2, Bh2 = h2_r.next()
                layer_norm(cx, pre[:], B_pre, 2, h2, Bh2)
                SS.dma("sp", lambda e: e.dma_start(out=H2[tsl, :], in_=h2[:]), reads=[Bh2], writes=[B_H2])
                h2b, Bh2b = h2b_r.next()
                SS.op("act", lambda e: e.copy(h2b[:], h2[:]), reads=[Bh2], writes=[Bh2b])
                cx.cur = (h2, Bh2, h2b, Bh2b)

            def st5(cx, ti):
                h2, Bh2, h2b, Bh2b = cx.cur
                big, B_big, h2T, B_h2T, sm, B_sm, rt, B_rt, Mb, B_Mb = (cx.big, cx.B_big, cx.h2T, cx.B_h2T, cx.sm, cx.B_sm,
                                                                       cx.rt, cx.B_rt, cx.Mb, cx.B_Mb)
                SS.op("pe", lambda e: [e.transpose(big[:, c * 128:(c + 1) * 128], h2[:, c * 128:(c + 1) * 128], ident_f[:])
                                      for c in range(8)][-1], reads=[Bh2, B_const], writes=[B_big])
                SS.op("act", lambda e: e.copy(h2T[:].rearrange("p a b -> p (a b)"), big[:]), reads=[B_big], writes=[B_h2T])
                SS.op("pe", mmchain([(rtp[:, 0:36], h2T[:, kc, :], wr[:, kc, :], kc == 0, kc == 7) for kc in range(8)]),
                     reads=[B_h2T, B_wr], writes=[B_rtp])
                R = [B_rt]
                SS.op("dve", lambda e: e.tensor_tensor(rt[:, 0:36], rtp[:, 0:36], brb[:], ALU.add), reads=[B_rtp, B_wr], writes=R)
                SS.op("dve", lambda e: e.reduce_max(sm[:, 8:9], rt[:, 0:4], axis=AX.X), reads=R, writes=[B_sm])
                SS.op("dve", lambda e: e.tensor_scalar(sm[:, 9:10], sm[:, 8:9], -1.0, None, op0=ALU.mult), reads=[B_sm], writes=[B_sm])
                SS.op("act", lambda e: e.activation(rt[:, 40:44], rt[:, 0:4], AF.Exp, bias=sm[:, 9:10], accum_out=sm[:, 10:11]),
                     reads=R + [B_sm], writes=R + [B_sm])
                SS.op("dve", lambda e: e.reciprocal(sm[:, 11:12], sm[:, 10:11]), reads=[B_sm], writes=[B_sm])
                SS.op("dve", lambda e: e.tensor_scalar(rt[:, 44:48], rt[:, 0:4], sm[:, 8:9], None, op0=ALU.is_equal),
                     reads=R + [B_sm], writes=R)
                SS.op("dve", lambda e: e.tensor_scalar(rt[:, 44:48], rt[:, 44:48], 1e30, -1e30, op0=ALU.mult, op1=ALU.add),
                     reads=R, writes=R)
                SS.op("dve", lambda e: e.tensor_tensor(rt[:, 64:96].rearrange("p (g e) -> p g e", g=4),
                                                      rt[:, 4:36].rearrange("p (g e) -> p g e", g=4),
                                                      rt[:, 44:48].unsqueeze(2).to_broadcast([128, 4, 8]), ALU.add),
                     reads=R, writes=R)
                SS.op("dve", lambda e: e.reduce_max(sm[:, 12:13], rt[:, 64:96], axis=AX.X), reads=R, writes=[B_sm])
                SS.op("dve", lambda e: e.tensor_scalar(rt[:, 96:128], rt[:, 64:96], sm[:, 12:13], None, op0=ALU.is_equal),
                     reads=R + [B_sm], writes=R)
                SS.op("dve", lambda e: e.scalar_tensor_tensor(rt[:, 128:160], rt[:, 96:128], -1e30, rt[:, 64:96],
                                                             op0=ALU.mult, op1=ALU.add), reads=R, writes=R)
                SS.op("dve", lambda e: e.reduce_max(sm[:, 13:14], rt[:, 128:160], axis=AX.X), reads=R, writes=[B_sm])
                SS.op("dve", lambda e: e.tensor_scalar(rt[:, 160:192], rt[:, 128:160], sm[:, 13:14], None, op0=ALU.is_equal),
                     reads=R + [B_sm], writes=R)
                SS.op("dve", lambda e: e.tensor_tensor(sm[:, 14:15], sm[:, 13:14], sm[:, 12:13], ALU.subtract),
                     reads=[B_sm], writes=[B_sm])
                SS.op("act", lambda e: e.activation(sm[:, 14:15], sm[:, 14:15], AF.Exp), reads=[B_sm], writes=[B_sm])
                SS.op("dve", lambda e: e.tensor_scalar(sm[:, 15:16], sm[:, 14:15], 1.0, None, op0=ALU.add), reads=[B_sm], writes=[B_sm])
                SS.op("dve", lambda e: e.reciprocal(sm[:, 15:16], sm[:, 15:16]), reads=[B_sm], writes=[B_sm])
                SS.op("dve", lambda e: e.tensor_tensor(GATE[:, ti, 0:1], sm[:, 15:16], sm[:, 11:12], ALU.mult),
                     reads=[B_sm], writes=[B_GATE])
                SS.op("dve", lambda e: e.tensor_tensor(GATE[:, ti, 1:2], GATE[:, ti, 0:1], sm[:, 14:15], ALU.mult),
                     reads=[B_sm, B_GATE], writes=[B_GATE])
                SS.op("dve", lambda e: e.tensor_copy(Mb[:, 0:32], rt[:, 96:128]), reads=R, writes=[B_Mb])
                SS.op("dve", lambda e: e.tensor_copy(Mb[:, 32:64], rt[:, 160:192]), reads=R, writes=[B_Mb])
                SS.op("pe", mmchain([(rtp[:, 64:128], ustrict[:], Mb[:], True, True),
                                    (rtp[:, 128:192], ones_b[:], Mb[:], True, True)]), reads=[B_Mb, B_const], writes=[B_rtp])
                SS.op("dve", lambda e: e.tensor_tensor(rt[:, 192:224], rtp[:, 64:96], cnt[:], ALU.add), reads=[B_rtp, B_cnt], writes=R)
                SS.op("dve", lambda e: e.tensor_tensor(rt[:, 224:256], rtp[:, 96:128], cnt[:], ALU.add), reads=[B_rtp, B_cnt], writes=R)
                SS.op("dve", lambda e: e.tensor_tensor(rt[:, 224:256], rt[:, 224:256], rtp[:, 128:160], ALU.add), reads=[B_rtp] + R, writes=R)
                SS.op("dve", lambda e: e.tensor_tensor(cnt[:], cnt[:], rtp[:, 128:160], ALU.add), reads=[B_rtp, B_cnt], writes=[B_cnt])
                SS.op("dve", lambda e: e.tensor_tensor(cnt[:], cnt[:], rtp[:, 160:192], ALU.add), reads=[B_rtp, B_cnt], writes=[B_cnt])
                for kk in range(2):
                    ohs = slice(96, 128) if kk == 0 else slice(160, 192)
                    rks = slice(192, 224) if kk == 0 else slice(224, 256)
                    SS.op("dve", lambda e, ohs=ohs, rks=rks: e.tensor_tensor(rt[:, rks], rt[:, rks], rt[:, ohs], ALU.mult), reads=R, writes=R)
                    SS.op("dve", lambda e, rks=rks: e.reduce_sum(sm[:, 6:7], rt[:, rks], axis=AX.X), reads=R, writes=[B_sm])
                    SS.op("dve", lambda e: e.tensor_scalar(sm[:, 6:7], sm[:, 6:7], float(CAP - 1), None, op0=ALU.min),
                         reads=[B_sm], writes=[B_sm])
                    SS.op("dve", lambda e, ohs=ohs, rks=rks: e.tensor_tensor(rt[:, rks], rt[:, ohs], ebase[:], ALU.mult),
                         reads=R + [B_wr], writes=R)
                    SS.op("dve", lambda e, rks=rks: e.reduce_sum(sm[:, 7:8], rt[:, rks], axis=AX.X), reads=R, writes=[B_sm])
                    SS.op("dve", lambda e: e.tensor_tensor(sm[:, 6:7], sm[:, 6:7], sm[:, 7:8], ALU.add), reads=[B_sm], writes=[B_sm])
                    SS.op("dve", lambda e, kk=kk: e.tensor_copy(DEST[:, ti, kk:kk + 1], sm[:, 6:7]), reads=[B_sm], writes=[B_DEST])
                    SS.dma("pool", lambda e, kk=kk: e.indirect_dma_start(
                        out=XG, out_offset=bass.IndirectOffsetOnAxis(ap=DEST[:, ti, kk:kk + 1], axis=0),
                        in_=h2b[:], in_offset=None), reads=[B_DEST, Bh2b], writes=[B_XG])

            stages = (st1, st2, st3, st4, st5)

            def replay_rec(it):
                kind, a_, k_ = it
                if kind == "op":
                    S.op(*a_, **k_)
                else:
                    S.dma(*a_, **k_)

            for pi in range(NT // 2):
                tA, tB = 2 * pi, 2 * pi + 1
                for sI in range(5):
                    if sI not in (0, 3):
                        stages[sI](cxs[0], tA)
                        stages[sI](cxs[1], tB)
                        continue
                    SS.rec = ra = []
                    stages[sI](cxs[0], tA)
                    SS.rec = rb = []
                    stages[sI](cxs[1], tB)
                    SS.rec = None
                    for kz in range(max(len(ra), len(rb))):
                        if kz < len(ra):
                            replay_rec(ra[kz])
                        if kz < len(rb):
                            replay_rec(rb[kz])
            if STOP == "p3f":
                return stop_here()
            if debug and n_phases == 3:
                d1 = dout("d_H2", [S_TOK, D])
                S.dma("sp", lambda e: e.dma_start(out=d1, in_=H2), reads=[B_H2])
                d2 = dout("d_DEST", [128, NT * 2], I32)
                S.dma("sp", lambda e: e.dma_start(out=d2, in_=DEST[:].rearrange("p a b -> p (a b)")), reads=[B_DEST])
                d3 = dout("d_GATE", [128, NT * 2])
                S.dma("sp", lambda e: e.dma_start(out=d3, in_=GATE[:].rearrange("p a b -> p (a b)")), reads=[B_GATE])
            S.flush()
        if n_phases <= 3:
            return nc, dbg

        with contextlib.ExitStack() as s4:
            wg_r = Rot([(sbt(s4, f"wg{i}", [128, 8, 512], BF16), Buf()) for i in range(3)])
            wu_r = Rot([(sbt(s4, f"wu{i}", [128, 8, 512], BF16), Buf()) for i in range(3)])
            wd_r = Rot([(sbt(s4, f"wd{i}", [128, 4, D], BF16), Buf()) for i in range(3)])
            xg_r = Rot([(sbt(s4, f"xg{i}", [128, 4, D], BF16), Buf()) for i in range(2)])
            xgT_r = Rot([(sbt(s4, f"xgT{i}", [128, 8, 512], BF16), Buf()) for i in range(2)])
            aT_r = Rot([(sbt(s4, f"aT{i}", [128, 4, 512], BF16), Buf()) for i in range(2)])
            sg_r = Rot([(sbt(s4, f"sgl{i}", [128, 512], F32), Buf()) for i in range(2)])
            yo_r = Rot([(sbt(s4, f"yo{i}", [128, D], F32), Buf()) for i in range(2)])
            ptp = Rot([(pst(s4, f"ptp{i}", [128, D], BF16), Buf()) for i in range(2)])

            pg_r = Rot([(pst(s4, f"pg{i}", [128, 512], F32), Buf()) for i in range(2)])
            pu_r = Rot([(pst(s4, f"pu{i}", [128, 512], F32), Buf()) for i in range(2)])
            py_r = Rot([(pst(s4, f"py{i}", [128, 512], F32), Buf()) for i in range(2)])
            exst = {}

            def p4_load(ex):
                wg, Bwg = wg_r.next()
                wu, Bwu = wu_r.next()
                wd, Bwd = wd_r.next()
                xg, Bxg = xg_r.next()
                S.dma("sp", lambda e: e.dma_start(out=xg[:], in_=XG[ex * CAP:(ex + 1) * CAP, :].rearrange("(b p) d -> p b d", p=128)),
                      reads=[B_XG], writes=[Bxg])
                S.dma("pool", lambda e: e.dma_start(out=wg[:], in_=w_gate[ex].rearrange("(kc p) n -> p kc n", p=128)), writes=[Bwg])
                S.dma("pool", lambda e: e.dma_start(out=wu[:], in_=w_up[ex].rearrange("(kc p) n -> p kc n", p=128)), writes=[Bwu])
                S.dma("pool", lambda e: e.dma_start(out=wd[:], in_=w_down[ex].rearrange("(kc p) n -> p kc n", p=128)), writes=[Bwd])
                exst[ex] = dict(wg=wg, Bwg=Bwg, wu=wu, Bwu=Bwu, wd=wd, Bwd=Bwd, xg=xg, Bxg=Bxg)

            def p4_transpose(ex):
                stt = exst[ex]
                xg, Bxg = stt["xg"], stt["Bxg"]
                xgT, B_xgT = xgT_r.next()
                stt["xgT"], stt["B_xgT"] = xgT, B_xgT
                for blk in range(4):
                    tp, Btp = ptp.next()
                    S.op("pe", lambda e, tp=tp, blk=blk: [e.transpose(tp[:, kc * 128:(kc + 1) * 128],
                                                                      xg[:, blk, kc * 128:(kc + 1) * 128], ident_b[:])
                                                          for kc in range(8)][-1], reads=[Bxg, B_const], writes=[Btp])
                    if blk % 2 == 0:
                        S.op("act", lambda e, tp=tp, blk=blk: e.copy(xgT[:, :, blk * 128:(blk + 1) * 128],
                                                                     tp[:].rearrange("p (a b) -> p a b", a=8)), reads=[Btp], writes=[B_xgT])
                    else:
                        S.op("dve", lambda e, tp=tp, blk=blk: e.tensor_copy(xgT[:, :, blk * 128:(blk + 1) * 128],
                                                                            tp[:].rearrange("p (a b) -> p a b", a=8)), reads=[Btp], writes=[B_xgT])

            def p4_gateup(ex):
                stt = exst[ex]
                wg, Bwg, wu, Bwu, xgT, B_xgT = stt["wg"], stt["Bwg"], stt["wu"], stt["Bwu"], stt["xgT"], stt["B_xgT"]
                aT, B_aT = aT_r.next()
                stt["aT"], stt["B_aT"] = aT, B_aT
                for fc in range(4):
                    pg, Bpg = pg_r.next()
                    pu, Bpu = pu_r.next()
                    S.op("pe", mmchain([(pg[:], wg[:, kc, fc * 128:(fc + 1) * 128], xgT[:, kc, :], kc == 0, kc == 7) for kc in range(8)]),
                         reads=[Bwg, B_xgT], writes=[Bpg])
                    S.op("pe", mmchain([(pu[:], wu[:, kc, fc * 128:(fc + 1) * 128], xgT[:, kc, :], kc == 0, kc == 7) for kc in range(8)]),
                         reads=[Bwu, B_xgT], writes=[Bpu])
                    sg, Bsg = sg_r.next()
                    S.op("act", lambda e, sg=sg, pg=pg: e.activation(sg[:], pg[:], AF.Silu), reads=[Bpg], writes=[Bsg])
                    S.op("dve", lambda e, sg=sg, pu=pu, fc=fc: e.tensor_tensor(aT[:, fc, :], sg[:], pu[:], ALU.mult),
                         reads=[Bsg, Bpu], writes=[B_aT])

            def p4_down(ex):
                stt = exst.pop(ex)
                aT, B_aT, wd, Bwd = stt["aT"], stt["B_aT"], stt["wd"], stt["Bwd"]
                for blk in range(4):
                    yo, Byo = yo_r.next()
                    for hf in range(2):
                        py, Bpy = py_r.next()
                        S.op("pe", mmchain([(py[:], aT[:, fc, blk * 128:(blk + 1) * 128], wd[:, fc, hf * 512:(hf + 1) * 512],
                                             fc == 0, fc == 3) for fc in range(4)]), reads=[B_aT, Bwd], writes=[Bpy])
                        if hf == 0:
                            S.op("act", lambda e, yo=yo, py=py: e.copy(yo[:, 0:512], py[:]), reads=[Bpy], writes=[Byo])
                        else:
                            S.op("dve", lambda e, yo=yo, py=py: e.tensor_copy(yo[:, 512:1024], py[:]), reads=[Bpy], writes=[Byo])
                    r0 = ex * CAP + blk * 128
                    S.dma("sp", lambda e, yo=yo, r0=r0: e.dma_start(out=YR[r0:r0 + 128, :], in_=yo[:]), reads=[Byo], writes=[B_YR])

            p4_load(0)
            p4_load(1)
            p4_transpose(0)
            for ex in range(NE):
                if ex + 2 < NE:
                    p4_load(ex + 2)
                p4_gateup(ex)
                if ex + 1 < NE:
                    p4_transpose(ex + 1)
                p4_down(ex)
            S.flush()
        if n_phases <= 4:
            return nc, dbg

        with contextlib.ExitStack() as s5:
            LN3 = sbt(s5, "LN3", [128, 2, D], F32)
            B_LN3 = Buf()
            for i in range(2):
                S.dma("sp", lambda e, i=i: e.dma_start(out=LN3[:, i, :], in_=lnp[4 + i:5 + i, :].partition_broadcast(128)), writes=[B_LN3])
            r1_r = Rot([(sbt(s5, f"r1_{i}", [128, D], F32), Buf()) for i in range(4)])
            r2_r = Rot([(sbt(s5, f"r2_{i}", [128, D], F32), Buf()) for i in range(4)])
            hh_r = Rot([(sbt(s5, f"hh_{i}", [128, D], F32), Buf()) for i in range(4)])
            pre5 = Rot([(sbt(s5, f"pre5_{i}", [128, D], F32), Buf()) for i in range(2)])
            xc5 = Rot([(sbt(s5, f"xc5_{i}", [128, D], F32), Buf()) for i in range(2)])
            sq5 = sbt(s5, "sq5", [128, D], F32)
            B_sq5 = Buf()
            o_r = Rot([(sbt(s5, f"o5_{i}", [128, D], F32), Buf()) for i in range(2)])
            sm5 = Rot([(sbt(s5, f"sm5_{i}", [128, 8], F32), Buf()) for i in range(2)])
            p5 = {}

            def p5_load(ti):
                tsl = slice(ti * 128, (ti + 1) * 128)
                r1, Br1 = r1_r.next()
                r2, Br2 = r2_r.next()
                hh, Bhh = hh_r.next()
                S.dma("pool", lambda e: e.indirect_dma_start(
                    out=r1[:], out_offset=None, in_=YR, in_offset=bass.IndirectOffsetOnAxis(ap=DEST[:, ti, 0:1], axis=0)),
                    reads=[B_YR, B_DEST], writes=[Br1])
                S.dma("pool", lambda e: e.indirect_dma_start(
                    out=r2[:], out_offset=None, in_=YR, in_offset=bass.IndirectOffsetOnAxis(ap=DEST[:, ti, 1:2], axis=0)),
                    reads=[B_YR, B_DEST], writes=[Br2])
                S.dma("sp", lambda e: e.dma_start(out=hh[:], in_=H2[tsl, :]), reads=[B_H2], writes=[Bhh])
                p5[ti] = (r1, Br1, r2, Br2, hh, Bhh)

            p5_load(0)
            p5_load(1)
            class Indir5:
                def __init__(self):
                    self.rec = None

                def op(self, *a_, **k_):
                    self.rec.append(("op", a_, k_))

                def dma(self, *a_, **k_):
                    self.rec.append(("dma", a_, k_))

            SS5 = Indir5()

            def p5_tile(ti):
                tsl = slice(ti * 128, (ti + 1) * 128)
                r1, Br1, r2, Br2, hh, Bhh = p5.pop(ti)
                pr, Bpr = pre5.next()
                SS5.op("dve", lambda e, pr=pr, r1=r1, ti=ti, hh=hh: e.tensor_scalar(pr[:], r1[:], GATE[:, ti, 0:1], None, op0=ALU.mult),
                     reads=[Br1, B_GATE], writes=[Bpr])
                SS5.op("dve", lambda e, pr=pr, r2=r2, ti=ti: e.scalar_tensor_tensor(pr[:], r2[:], GATE[:, ti, 1:2], pr[:],
                                                                                  op0=ALU.mult, op1=ALU.add),
                       reads=[Br2, Bpr, B_GATE], writes=[Bpr])
                SS5.op("dve", lambda e, pr=pr, hh=hh: e.scalar_tensor_tensor(pr[:], hh[:], ALPHA, pr[:], op0=ALU.mult, op1=ALU.add),
                     reads=[Bhh, Bpr], writes=[Bpr])
                s5m, Bs5 = sm5.next()
                xc, Bxc = xc5.next()
                SS5.op("dve", lambda e, s5m=s5m, pr=pr: e.reduce_sum(s5m[:, 0:1], pr[:], axis=AX.X), reads=[Bpr], writes=[Bs5])
                SS5.op("dve", lambda e, s5m=s5m: e.tensor_scalar(s5m[:, 1:2], s5m[:, 0:1], -1.0 / D, None, op0=ALU.mult), reads=[Bs5], writes=[Bs5])
                SS5.op("act", lambda e, xc=xc, pr=pr, s5m=s5m: e.activation(xc[:], pr[:], AF.Identity, bias=s5m[:, 1:2]),
                     reads=[Bpr, Bs5], writes=[Bxc])
                SS5.op("act", lambda e, xc=xc, s5m=s5m: e.activation(sq5[:], xc[:], AF.Square, accum_out=s5m[:, 2:3]),
                     reads=[Bxc], writes=[B_sq5, Bs5])
                SS5.op("dve", lambda e, s5m=s5m: e.tensor_scalar(s5m[:, 3:4], s5m[:, 2:3], 1.0 / D, EPS, op0=ALU.mult, op1=ALU.add),
                     reads=[Bs5], writes=[Bs5])
                SS5.op("act", lambda e, s5m=s5m: e.activation(s5m[:, 4:5], s5m[:, 3:4], AF.Sqrt), reads=[Bs5], writes=[Bs5])
                SS5.op("dve", lambda e, s5m=s5m: e.reciprocal(s5m[:, 5:6], s5m[:, 4:5]), reads=[Bs5], writes=[Bs5])
                SS5.op("dve", lambda e, xc=xc, s5m=s5m: e.scalar_tensor_tensor(xc[:], xc[:], s5m[:, 5:6], LN3[:, 0, :], op0=ALU.mult, op1=ALU.mult),
                     reads=[Bxc, Bs5, B_LN3], writes=[Bxc])
                oo, Boo = o_r.next()
                SS5.op("pool", lambda e, oo=oo, xc=xc: e.tensor_tensor(oo[:], xc[:], LN3[:, 1, :], ALU.add), reads=[Bxc, B_LN3], writes=[Boo])
                SS5.dma("sp", lambda e, oo=oo, tsl=tsl: e.dma_start(out=out[tsl, :], in_=oo[:]), reads=[Boo])

            for pi in range(NT // 2):
                recs = []
                for tn in (2 * pi + 2, 2 * pi + 3):
                    if tn < NT:
                        p5_load(tn)
                for ti in (2 * pi, 2 * pi + 1):
                    SS5.rec = []
                    p5_tile(ti)
                    recs.append(SS5.rec)
                for kz in range(max(len(recs[0]), len(recs[1]))):
                    for rr_ in recs:
                        if kz < len(rr_):
                            kind, a_, k_ = rr_[kz]
                            (S.op if kind == "op" else S.dma)(*a_, **k_)
            S.flush()
    return nc, dbg


def _prep_shared(inputs):
    f = lambda a: np.ascontiguousarray(np.asarray(a, dtype=np.float32))
    w_in = f(inputs["w_in"])[0]
    offs = np.cumsum([0, 512, 512, 512, 8, 512, 128, 128])
    qf = w_in[:, offs[0]:offs[1]]
    kf = w_in[:, offs[1]:offs[2]]
    vf = w_in[:, offs[2]:offs[3]]
    fl = w_in[:, offs[3]:offs[4]]
    qs = w_in[:, offs[4]:offs[5]]
    ks = w_in[:, offs[5]:offs[6]]
    vs = w_in[:, offs[6]:offs[7]]

    def swap_halves(w, nh):
        w4 = w.reshape(w.shape[0], nh, 2, 32)
        return w4[:, :, ::-1, :].reshape(w.shape[0], nh * 64)

    w_fm = np.concatenate([qf, kf, qs, swap_halves(qs, 8), ks, swap_halves(ks, 2)], axis=1)
    w_tm = np.concatenate([vf, vs, fl], axis=1)
    lnp = np.stack([f(inputs[k])[0] for k in ("ln_mix_g", "ln_mix_b", "ln_x_g", "ln_x_b", "ln_ffn_g", "ln_ffn_b")], axis=0)
    w_re = f(inputs["w_route_expert"])[0]
    w_r = np.concatenate([f(inputs["w_route_group"])[0], w_re.transpose(1, 0, 2).reshape(D, 32)], axis=1)
    b_r = np.concatenate([f(inputs["b_route_group"])[0], f(inputs["b_route_expert"])[0].reshape(32)])[None, :]
    half = 32
    inv_freq = (10000.0 ** (-np.arange(half, dtype=np.float32) / half)).astype(np.float32)
    p = np.arange(128)
    cinv = inv_freq[p % 32].reshape(128, 1).astype(np.float32)
    csgn = np.where((p % 64) < 32, -1.0, 1.0).reshape(128, 1).astype(np.float32)
    return dict(
        w_fm=np.ascontiguousarray(w_fm), w_tm=np.ascontiguousarray(w_tm),
        b_forget=f(inputs["b_forget"]), sinks=f(inputs["sinks"]),
        w_mix_out=f(inputs["w_mix_out"])[0], w_xq=f(inputs["w_xq"])[0], w_xkv=f(inputs["w_xkv"])[0],
        w_xout=f(inputs["w_xout"])[0], lnp=np.ascontiguousarray(lnp), w_r=np.ascontiguousarray(w_r),
        b_r=np.ascontiguousarray(b_r.astype(np.float32)),
        w_gate=f(inputs["w_exp_gate"])[0], w_up=f(inputs["w_exp_up"])[0], w_down=f(inputs["w_exp_down"])[0],
        cinv=cinv, csgn=csgn)


def make_in_maps(inputs, cores, names=None):
    shared = _prep_shared(inputs)
    x = np.asarray(inputs["x"], dtype=np.float32)
    mem = np.asarray(inputs["mem"], dtype=np.float32)
    positions = np.asarray(inputs["positions"]).astype(np.int32)
    maps = []
    for b in cores:
        m = dict(shared)
        m["xT"] = np.ascontiguousarray(x[b].T)
        m["x_tok"] = np.ascontiguousarray(x[b])
        m["memT"] = np.ascontiguousarray(mem[b].T)
        m["pos"] = np.ascontiguousarray(positions[b][None, :])
        if names is not None:
            m = {k: v for k, v in m.items() if k in names}
        maps.append(m)
    return maps


_NC_CACHE = {}


def kernel(**inputs):
    if "nc" not in _NC_CACHE:
        _NC_CACHE["nc"] = build_program()[0]
    nc = _NC_CACHE["nc"]
    in_maps = make_in_maps(inputs, list(range(8)))
    res = run_bass_kernel_spmd(nc, in_maps, core_ids=list(range(8)))
    outp = np.stack([np.asarray(r["out"], dtype=np.float32) for r in res.results], axis=0)
    return outp
```
